# Optimizing a Trainium2 kernel written in Bass

```python
import math
import jax, jax.numpy as jnp
from jax import lax
import numpy as np

D_MODEL = 4096
BATCH = 8
SEQ = 2048
DEPTH = 1

D_MIX = D_MODEL
D_HG = D_MIX // 2
D_SB = D_MIX - D_HG
HEAD_DIM = 128
HG_HEADS = D_HG // HEAD_DIM
SB_HEADS = D_SB // HEAD_DIM
D_IN_PROJ = 4 * D_HG + 3 * D_SB
HG_CHUNK = 64
SB_BLOCK = 128
N_EXPERTS = 32
TOP_K = 4
D_FF = 3 * D_MODEL // 8
MOE_BLOCK = 128
SWIGLU_ALPHA = 1.702
SWIGLU_LIMIT = 7.0
NORM_EPS = 1e-6

kernel_name = "hybrid_hgrn2_stickbreaking_moe_layer"


def rmsnorm(t, w):
    tf = t.astype(jnp.float32)
    tf = tf * lax.rsqrt(jnp.mean(tf * tf, axis=-1, keepdims=True) + NORM_EPS)
    return (tf * w.astype(jnp.float32)).astype(t.dtype)


def hgrn2_mixer(q, f_logit, v, g, lb, norm_w):
    B_, S_, _ = q.shape
    nc = S_ // HG_CHUNK
    lb = lb.astype(jnp.float32)
    f = lb + (1.0 - lb) * jax.nn.sigmoid(f_logit.astype(jnp.float32))
    log_f = jnp.log(f)
    k = 1.0 - f

    def to_chunks(t):
        t = t.astype(jnp.float32).reshape(B_, nc, HG_CHUNK, HG_HEADS, HEAD_DIM)
        return t.transpose(1, 0, 3, 2, 4)

    causal = jnp.tril(jnp.ones((HG_CHUNK, HG_CHUNK), dtype=bool))[:, :, None]

    def step(state, inp):
        qc, kc, vc, lfc = inp
        b = jnp.cumsum(lfc, axis=2)
        o_inter = jnp.einsum('bhtk,bhkv->bhtv', qc * jnp.exp(b), state)
        rel = b[:, :, :, None, :] - b[:, :, None, :, :]
        decay = jnp.exp(jnp.where(causal, rel, -jnp.inf))
        scores = jnp.einsum('bhtk,bhtsk,bhsk->bhts', qc, decay, kc)
        o = o_inter + jnp.einsum('bhts,bhsv->bhtv', scores, vc)
        b_last = b[:, :, -1:, :]
        new_state = state * jnp.exp(b_last[:, :, 0, :, None]) + jnp.einsum(
            'bhsk,bhsv->bhkv', kc * jnp.exp(b_last - b), vc)
        return new_state, o

    state0 = jnp.zeros((B_, HG_HEADS, HEAD_DIM, HEAD_DIM), jnp.float32)
    _, o = lax.scan(step, state0, (to_chunks(q), to_chunks(k), to_chunks(v), to_chunks(log_f)))
    o = o.transpose(1, 0, 3, 2, 4).reshape(B_, S_, HG_HEADS, HEAD_DIM)
    o = o * lax.rsqrt(jnp.mean(o * o, axis=-1, keepdims=True) + NORM_EPS) * norm_w.astype(jnp.float32)
    o = o.reshape(B_, S_, D_HG) * jax.nn.silu(g.astype(jnp.float32))
    return o.astype(q.dtype)


def stick_breaking_attention(q, k, v):
    S_ = q.shape[2]
    scale = HEAD_DIM ** -0.5
    outs = []
    for i in range(S_ // SB_BLOCK):
        lo, hi = i * SB_BLOCK, (i + 1) * SB_BLOCK
        qb, kb, vb = q[:, :, lo:hi], k[:, :, :hi], v[:, :, :hi]
        z = jnp.einsum('bhtd,bhsd->bhts', qb, kb).astype(jnp.float32) * scale
        strict = jnp.arange(hi)[None, :] < (lo + jnp.arange(SB_BLOCK))[:, None]
        log_keep = jnp.where(strict, -jax.nn.softplus(z), 0.0)
        log_w = z + lax.cumsum(log_keep, axis=3, reverse=True)
        w = jnp.exp(jnp.where(strict, log_w, -jnp.inf))
        outs.append(jnp.einsum('bhts,bhsd->bhtd', w.astype(vb.dtype), vb))
    return jnp.concatenate(outs, axis=2)


def hybrid_mixer(h, w_in, lb, hg_norm_w, w_out):
    B_, S_, _ = h.shape
    proj = h @ w_in
    splits = [D_HG, 2 * D_HG, 3 * D_HG, 4 * D_HG, 4 * D_HG + D_SB, 4 * D_HG + 2 * D_SB]
    hq, hf, hv, hgate, sq, sk, sv = jnp.split(proj, splits, axis=-1)
    o_hg = hgrn2_mixer(hq, hf, hv, hgate, lb, hg_norm_w)

    def heads(t):
        return t.reshape(B_, S_, SB_HEADS, HEAD_DIM).transpose(0, 2, 1, 3)
    o_sb = stick_breaking_attention(heads(sq), heads(sk), heads(sv))
    o_sb = o_sb.transpose(0, 2, 1, 3).reshape(B_, S_, D_SB)
    return jnp.concatenate([o_hg, o_sb.astype(o_hg.dtype)], axis=-1) @ w_out


def clamped_swiglu(glu, lin):
    glu = jnp.minimum(glu, SWIGLU_LIMIT)
    lin = jnp.clip(lin, -SWIGLU_LIMIT, SWIGLU_LIMIT)
    return glu * jax.nn.sigmoid(SWIGLU_ALPHA * glu) * (lin + 1.0)


def moe_ffn(h, w_router, b_router, w_gu, b_gu, w_down, b_down):
    B_, S_, D = h.shape
    T = B_ * S_
    ht = h.reshape(T, D)
    logits = (ht @ w_router).astype(jnp.float32) + b_router.astype(jnp.float32)
    top_logit, top_idx = lax.top_k(logits, TOP_K)
    gates = jax.nn.softmax(top_logit, axis=-1)

    n_assign = T * TOP_K
    flat_e = top_idx.reshape(-1)
    flat_tok = jnp.arange(n_assign, dtype=jnp.int32) // TOP_K
    flat_g = gates.reshape(-1)
    order = jnp.argsort(flat_e)
    sorted_e = flat_e[order]
    counts = jnp.bincount(flat_e, length=N_EXPERTS)
    padded = (counts + MOE_BLOCK - 1) // MOE_BLOCK * MOE_BLOCK
    pad_end = jnp.cumsum(padded)
    pad_start = pad_end - padded
    start = jnp.cumsum(counts) - counts
    dest = pad_start[sorted_e] + jnp.arange(n_assign, dtype=jnp.int32) - start[sorted_e]
    n_rows = -(-n_assign // MOE_BLOCK) * MOE_BLOCK + N_EXPERTS * MOE_BLOCK
    n_blocks = n_rows // MOE_BLOCK
    row_tok = jnp.zeros((n_rows,), jnp.int32).at[dest].set(flat_tok[order])
    row_gate = jnp.zeros((n_rows,), jnp.float32).at[dest].set(flat_g[order])
    blk_expert = jnp.minimum(
        jnp.searchsorted(pad_end, jnp.arange(n_blocks, dtype=pad_end.dtype) * MOE_BLOCK, side='right'),
        N_EXPERTS - 1)

    def body(acc, blk):
        tok, gate, e = blk
        xb = ht[tok]
        gu = xb @ w_gu[e] + b_gu[e]
        glu, lin = jnp.split(gu, 2, axis=-1)
        y = clamped_swiglu(glu, lin) @ w_down[e] + b_down[e]
        return acc.at[tok].add(gate[:, None] * y.astype(jnp.float32)), None

    out, _ = lax.scan(body, jnp.zeros((T, D), jnp.float32),
                      (row_tok.reshape(n_blocks, MOE_BLOCK), row_gate.reshape(n_blocks, MOE_BLOCK), blk_expert))
    return out.reshape(B_, S_, D).astype(h.dtype)


def setup_inputs(seed: int = 0) -> dict:
    key = jax.random.key(seed)
    ks = jax.random.split(key, 18)
    f32 = jnp.float32
    nrm = lambda k, shape, s: jax.random.normal(k, shape, f32) * s
    return {
        "x": nrm(ks[0], (BATCH, SEQ, D_MODEL), 1.0),
        "c": nrm(ks[1], (BATCH, D_MODEL), 1.0),
        "w_mod": nrm(ks[2], (DEPTH, D_MODEL, 6 * D_MODEL), 0.5 * D_MODEL ** -0.5),
        "b_mod": nrm(ks[3], (DEPTH, 6 * D_MODEL), 0.01),
        "mix_pre_norm": 1.0 + nrm(ks[4], (DEPTH, D_MODEL), 0.1),
        "mix_post_norm": 1.0 + nrm(ks[5], (DEPTH, D_MODEL), 0.1),
        "w_in": nrm(ks[6], (DEPTH, D_MODEL, D_IN_PROJ), D_MODEL ** -0.5),
        "hg_lb": nrm(ks[7], (DEPTH + 1, D_HG), 0.1),
        "hg_norm_w": 1.0 + nrm(ks[8], (DEPTH, HEAD_DIM), 0.1),
        "w_out": nrm(ks[9], (DEPTH, D_MIX, D_MODEL), D_MIX ** -0.5),
        "ffn_pre_norm": 1.0 + nrm(ks[10], (DEPTH, D_MODEL), 0.1),
        "ffn_post_norm": 1.0 + nrm(ks[11], (DEPTH, D_MODEL), 0.1),
        "w_router": nrm(ks[12], (DEPTH, D_MODEL, N_EXPERTS), D_MODEL ** -0.5),
        "b_router": nrm(ks[13], (DEPTH, N_EXPERTS), 0.01),
        "w_gu": nrm(ks[14], (DEPTH, N_EXPERTS, D_MODEL, 2 * D_FF), D_MODEL ** -0.5),
        "b_gu": nrm(ks[15], (DEPTH, N_EXPERTS, 2 * D_FF), 0.01),
        "w_down": nrm(ks[16], (DEPTH, N_EXPERTS, D_FF, D_MODEL), D_FF ** -0.5),
        "b_down": nrm(ks[17], (DEPTH, N_EXPERTS, D_MODEL), 0.01),
    }


def reference(x, c, w_mod, b_mod, mix_pre_norm, mix_post_norm, w_in, hg_lb, hg_norm_w, w_out,
              ffn_pre_norm, ffn_post_norm, w_router, b_router, w_gu, b_gu, w_down, b_down):
    lb_table = jnp.cumsum(jax.nn.softmax(hg_lb.astype(jnp.float32), axis=0), axis=0)
    c_act = jax.nn.silu(c)
    for l in range(DEPTH):
        mod = (c_act @ w_mod[l] + b_mod[l])[:, None, :]
        sh1, sc1, g1, sh2, sc2, g2 = jnp.split(mod, 6, axis=-1)
        h = rmsnorm(x, mix_pre_norm[l]) * (1.0 + sc1) + sh1
        y = hybrid_mixer(h, w_in[l], lb_table[l], hg_norm_w[l], w_out[l])
        x = x + g1 * rmsnorm(y, mix_post_norm[l])
        h = rmsnorm(x, ffn_pre_norm[l]) * (1.0 + sc2) + sh2
        y = moe_ffn(h, w_router[l], b_router[l], w_gu[l], b_gu[l], w_down[l], b_down[l])
        x = x + g2 * rmsnorm(y, ffn_post_norm[l])
    return x
```

```python
import os
import numpy as np
from contextlib import ExitStack
import concourse.bass as bass
import concourse.mybir as mybir
from concourse.bass_utils import run_bass_kernel_spmd

F32 = mybir.dt.float32
BF16 = mybir.dt.bfloat16
AF = mybir.ActivationFunctionType
ALU = mybir.AluOpType

D = 4096
S = 2048
NB = 8
NE = 32
DFF = 1536
EPS = 1e-6
SCALE = 128 ** -0.5
NTB = S // 128
KC = D // 128


class Buf:
    __slots__ = ("name", "w", "r")

    def __init__(self, name):
        self.name = name
        self.w = None
        self.r = []


class Sched:
    NDMA = 48

    def __init__(self, nc, es):
        self.nc = nc
        self.es = es
        self.eng = {"pe": nc.tensor, "act": nc.scalar, "dve": nc.vector,
                    "pool": nc.gpsimd, "sp": nc.sync}
        self.sems = {}
        self.cnt = {}
        for k in ("pe", "act", "dve", "pool", "cc"):
            self.sems[k] = es.enter_context(nc.semaphore("s_" + k))
            self.cnt[k] = 0
        for i in range(self.NDMA):
            k = "d%d" % i
            self.sems[k] = es.enter_context(nc.semaphore(k))
            self.cnt[k] = 0
        self.dnext = 0
        self.waited = {k: {} for k in self.eng}
        self.nins = 0
        self.trace = [] if os.environ.get("MK_TRACE") else None

    def _wait(self, ek, deps):
        e = self.eng[ek]
        best = {}
        for d in deps:
            if d is None:
                continue
            sk, v = d
            if sk == "pe" and ek == "pe":
                continue
            if v > best.get(sk, 0):
                best[sk] = v
        for sk, v in best.items():
            if self.waited[ek].get(sk, 0) >= v:
                continue
            e.wait_ge(self.sems[sk], v)
            self.waited[ek][sk] = v
            self.nins += 1
            if self.trace is not None:
                self.trace.append("   %s WAIT %s>=%d" % (ek, sk, v))

    @staticmethod
    def _deps(reads, writes):
        deps = []
        for b in reads:
            deps.append(b.w)
        for b in writes:
            deps.append(b.w)
            deps.extend(b.r)
        return deps

    @staticmethod
    def _mark(tok, reads, writes):
        for b in reads:
            b.r.append(tok)
            if len(b.r) > 64:
                best = {}
                for sk, v in b.r:
                    if v > best.get(sk, 0):
                        best[sk] = v
                b.r = list(best.items())
        for b in writes:
            b.w = tok
            b.r = []

    def op(self, ek, fn, reads=(), writes=(), track=True):
        self._wait(ek, self._deps(reads, writes))
        ins = fn(self.eng[ek])
        self.nins += 1
        if track:
            self.cnt[ek] += 1
            ins.then_inc(self.sems[ek], 1)
            tok = (ek, self.cnt[ek])
        else:
            tok = (ek, self.cnt[ek] + 1)
        if self.trace is not None:
            import sys as _s
            self.trace.append("%s OP line %d -> %s=%d%s  R=%s W=%s" % (ek, _s._getframe(1).f_lineno, tok[0], tok[1], "" if track else "(untracked)",
                                                              [b.name for b in reads], [b.name for b in writes]))
        self._mark(tok, reads, writes)
        return ins

    def dma(self, qk, fn, reads=(), writes=()):
        i = self.dnext
        self.dnext = (self.dnext + 1) % self.NDMA
        sk = "d%d" % i
        deps = self._deps(reads, writes)
        if self.cnt[sk] > 0:
            deps.append((sk, self.cnt[sk]))
        self._wait(qk, deps)
        ins = fn(self.eng[qk])
        self.nins += 1
        self.cnt[sk] += 16
        ins.then_inc(self.sems[sk], 16)
        self._mark((sk, self.cnt[sk]), reads, writes)
        if self.trace is not None:
            import sys as _s
            self.trace.append("%s DMA line %d -> %s=%d  R=%s W=%s" % (qk, _s._getframe(1).f_lineno, sk, self.cnt[sk],
                                                                   [b.name for b in reads], [b.name for b in writes]))
        return ins

    def coll(self, fn, reads=(), writes=()):
        self._wait("pool", self._deps(reads, writes))
        ins = fn(self.eng["pool"])
        self.cnt["cc"] += 1
        ins.then_inc(self.sems["cc"], 1)
        self._mark(("cc", self.cnt["cc"]), reads, writes)
        return ins

    def barrier(self):
        toks = [(k, v) for k, v in self.cnt.items() if v > 0]
        for ek in self.eng:
            self._wait(ek, toks)

    def finish(self, bufs, ek="sp"):
        self._wait(ek, [b.w for b in bufs])


def build(NR, dbg=False):
    nc = bass.Bass("TRN2", target_bir_lowering=False)
    KCS = KC // NR
    di = lambda name, shape, dt=F32: nc.dram_tensor(name, list(shape), dt, kind="ExternalInput").ap()
    dn = lambda name, shape, dt=BF16: nc.dram_tensor(name, list(shape), dt, kind="Internal").ap()

    x = di("x", [S, D])
    cT = di("cT", [128, KCS, NB])
    w_mod = di("w_mod", [KCS * 128, 6 * D])
    b_mod = di("b_mod", [1, 6 * D])
    oh = di("oh", [NR * NB + 1, 128])
    n_pre = di("n_pre", [128, D])
    n_post = di("n_post", [128, D])
    n_fpre = di("n_fpre", [128, D])
    n_fpost = di("n_fpost", [128, D])
    w_in = di("w_in", [112 // NR, 128, KC, 128])
    w_out = di("w_out", [8 // NR, 128, KC, 512])
    hg_lb = di("hg_lb", [128, 2, 16])
    hg_nw = di("hg_nw", [128, 1])
    w_r = di("w_r", [128, KC, NE])
    b_r = di("b_r", [128, NE])
    NEXP = int(os.environ.get("MK_NEXP", NE))
    NELD = 1 if os.environ.get("MK_SKIP_MOE") else (NE // NR if NEXP == NE else max(1, NEXP // NR))
    w_gu = di("w_gu", [NELD, 24, 128, KC, 128])
    b_gu = di("b_gu", [128, NE, 24])
    w_dn = di("w_dn", [NELD, 8, 128, 12, 512])
    b_dn = di("b_dn", [NE, D])
    consts = di("consts", [128, 1152])
    out = nc.dram_tensor("out", [S, D], F32, kind="ExternalOutput").ap()
    dbgout = {}
    if dbg:
        for nm, shp in (("d_mod", [128, 6 * D]), ("d_h1T", [128, KC, S]), ("d_oT", [32, 128, S]),
                        ("d_x1", [S, D]), ("d_G", [128, NTB, NE]), ("d_y", [S, D])):
            dbgout[nm] = nc.dram_tensor(nm, shp, F32, kind="ExternalOutput").ap()

    win_s = dn("win_s", [112 // NR, 128, KC, 128])
    wout_s = dn("wout_s", [8 // NR, 128, KC, 512])
    wgu_s = [dn("wgu_s%d" % i, [24, 128, KC, 128]) for i in range(NE // NR)]
    wdn_s = [dn("wdn_s%d" % i, [8, 128, 12, 512]) for i in range(NE // NR)]
    if NR > 1:
        win_b = dn("win_b", [112, 128, KC, 128])
        wout_b = dn("wout_b", [8, 128, KC, 512])
        wgu_b = [dn("wgu_b%d" % i, [NR, 24, 128, KC, 128]) for i in range(NE // NR)]
        wdn_b = [dn("wdn_b%d" % i, [NR, 8, 128, 12, 512]) for i in range(NE // NR)]
        modp_s = dn("modp_s", [NB, 6 * D], F32)
        modp = dn("modp", [NR * NB, 6 * D], F32)
    else:
        win_b, wout_b = win_s, wout_s
        modp_s = dn("modp_s", [NB, 6 * D], F32)
        modp = modp_s
    MODB = dn("MODB", [128, 6 * D], F32)
    OT = dn("OT", [32, 128, S], BF16)
    Y = dn("Y", [S, D], F32)
    X1 = dn("X1", [S, D], F32)
    H2T = dn("H2T", [128, KC, S], BF16)
    G2WD = dn("G2WD", [128, D], F32)
    groups = [list(range(g * NR, (g + 1) * NR)) for g in range(8 // NR)] if NR > 1 else None

    with ExitStack() as es:
        Sx = Sched(nc, es)
        op, dma = Sx.op, Sx.dma

        def sb(es_, name, shape, dt):
            return es_.enter_context(nc.sbuf_tensor(name, list(shape), dt))

        PS = [es.enter_context(nc.psum_tensor("ps%d" % i, [128, 512], F32)) for i in range(7)]
        PSB = [Buf("ps%d" % i) for i in range(7)]
        PTR = es.enter_context(nc.psum_tensor("ptr", [128, 8, 128], BF16))
        b_PTR = Buf("ptr")

        cst = sb(es, "cst", [128, 1152], F32)
        b_cst = Buf("cst")
        dma("sp", lambda e: e.dma_start(out=cst[:], in_=consts), writes=[b_cst])
        ident_f = cst[:, 0:128]
        mask01 = cst[:, 128:256]
        utri = cst[:, 256:384]
        ones_f = cst[:, 384:512]
        bigm_f = cst[:, 512:640]
        mask2 = cst[:, 640:768]
        o128 = cst[:, 768:896]
        rmask_f = cst[:, 896:1152]
        cb16 = sb(es, "cb16", [128, 256], BF16)
        b_cb16 = Buf("cb16")
        op("dve", lambda e: e.tensor_copy(out=cb16[:, 0:128], in_=ident_f), reads=[b_cst], writes=[b_cb16])
        op("dve", lambda e: e.tensor_copy(out=cb16[:, 128:256], in_=bigm_f), reads=[b_cst], writes=[b_cb16])
        ident_b = cb16[:, 0:128]
        bigm_b = cb16[:, 128:256]

        b_win, b_wout = Buf("win"), Buf("wout")
        b_wgu = [Buf("wgu%d" % i) for i in range(NE // NR)]
        b_wdn = [Buf("wdn%d" % i) for i in range(NE // NR)]

        def cast2d(dst, src, nel, wb):
            rows = nel // 2048
            d2 = dst.tensor.reshape([rows, 2048]) if False else None
            step = 8192
            r0 = 0
            while r0 < rows:
                r1 = min(rows, r0 + step)
                dma("pool", lambda e, r0=r0, r1=r1: e.dma_start(out=dst[r0:r1, :], in_=src[r0:r1, :]), writes=(wb if isinstance(wb, list) else [wb]))
                r0 = r1

        def flat(ap, nel):
            names = " ".join("a%d" % i for i in range(len(ap.shape)))
            f = ap.rearrange("%s -> (%s)" % (names, names))
            return f.rearrange("(r c) -> r c", c=2048)

        AGR = 192
        if NR > 1:
            agst = [dn("agst%d" % k, [NR * AGR, 2048]) for k in range(2)]
            b_agst = [Buf("agst0"), Buf("agst1")]
        agctr = [0]

        def ag_chunked(src2, dst2, rows, rbufs, wbufs):
            dst3 = dst2.rearrange("(r n) c -> r n c", r=NR)
            c0 = 0
            while c0 < rows:
                n = min(AGR, rows - c0)
                k = agctr[0] % 2
                agctr[0] += 1
                Sx.coll(lambda e: e.collective_compute("AllGather", ALU.bypass, replica_groups=groups,
                                                       ins=[src2[c0:c0 + n, :]], outs=[agst[k][0:NR * n, :]]),
                        reads=rbufs, writes=[b_agst[k]])
                dma("sp", lambda e: e.dma_start(out=dst3[:, c0:c0 + n, :],
                                                in_=agst[k][0:NR * n, :].rearrange("(r n) c -> r n c", r=NR)),
                    reads=[b_agst[k]], writes=wbufs)
                c0 += n

        with ExitStack() as e0:
            Sx.barrier()
            cTt = sb(e0, "cTt", [128, KCS, NB], F32)
            b_cT = Buf("cT")
            dma("sp", lambda e: e.dma_start(out=cTt[:], in_=cT), writes=[b_cT])
            sg = sb(e0, "sgc", [128, KCS, NB], F32)
            op("act", lambda e: e.activation(out=sg[:], in_=cTt[:], func=AF.Sigmoid), reads=[b_cT], writes=[b_cT])
            op("dve", lambda e: e.tensor_tensor(out=cTt[:], in0=cTt[:], in1=sg[:], op=ALU.mult), reads=[b_cT], writes=[b_cT])
            if os.environ.get("MK_STOP") == "0a":
                Sx.finish([b_cT, b_cb16], "sp"); build.sched = Sx; return nc
            wm = [sb(e0, "wm%d" % i, [128, KCS, 512], F32) for i in range(2)]
            b_wm = [Buf("wm0"), Buf("wm1")]
            mst = sb(e0, "mst", [NB, 512], F32)
            b_mst = Buf("mst")
            b_modp = Buf("modp")
            for cg in range(48):
                i = cg % 2
                dma("sp" if i == 0 else "act", lambda e, cg=cg, i=i: e.dma_start(
                    out=wm[i][:], in_=w_mod[:, cg * 512:(cg + 1) * 512].rearrange("(kc p) n -> p kc n", p=128)),
                    writes=[b_wm[i]])
                for kc in range(KCS):
                    op("pe", lambda e, kc=kc, i=i: e.matmul(PS[0][0:NB, :], lhsT=cTt[:, kc, :], rhs=wm[i][:, kc, :],
                                                           start=(kc == 0), stop=(kc == KCS - 1)),
                       reads=[b_cT, b_wm[i]], writes=[PSB[0]], track=(kc == KCS - 1))
                op("act", lambda e: e.copy(out=mst[:], in_=PS[0][0:NB, :]), reads=[PSB[0]], writes=[b_mst])
                dma("sp", lambda e, cg=cg: e.dma_start(out=modp_s[:, cg * 512:(cg + 1) * 512], in_=mst[:]),
                    reads=[b_mst], writes=[b_modp])
            if os.environ.get("MK_STOP") == "0b":
                Sx.finish([b_modp], "sp"); build.sched = Sx; return nc
            if NR > 1:
                Sx.coll(lambda e: e.collective_compute("AllGather", ALU.bypass, replica_groups=groups,
                                                       ins=[modp_s], outs=[modp]), reads=[b_modp], writes=[b_modp])
            KR = NR * NB + 1
            KP = 32 if KR <= 32 else 64
            mp = sb(e0, "mp", [KP, 2048], F32)
            oht = sb(e0, "oht", [KP, 128], F32)
            b_mp, b_oh = Buf("mp"), Buf("oh")
            op("dve", lambda e: e.memset(mp[:], 0.0), writes=[b_mp])
            op("dve", lambda e: e.memset(oht[:], 0.0), writes=[b_oh])
            dma("sp", lambda e: e.dma_start(out=oht[0:KR, :], in_=oh), writes=[b_oh])
            mbs = sb(e0, "mbs", [128, 2048], F32)
            b_mbs = Buf("mbs")
            b_MODB = Buf("MODB")
            for q in range(12):
                dma("sp", lambda e, q=q: e.dma_start(out=mp[0:KR - 1, :], in_=modp[:, q * 2048:(q + 1) * 2048]),
                    reads=[b_modp], writes=[b_mp])
                dma("act", lambda e, q=q: e.dma_start(out=mp[KR - 1:KR, :], in_=b_mod[:, q * 2048:(q + 1) * 2048]),
                    writes=[b_mp])
                for j in range(4):
                    op("pe", lambda e, j=j: e.matmul(PS[1][:, :], lhsT=oht[:, :], rhs=mp[:, j * 512:(j + 1) * 512],
                                                     start=True, stop=True), reads=[b_mp, b_oh], writes=[PSB[1]])
                    op("act", lambda e, j=j: e.copy(out=mbs[:, j * 512:(j + 1) * 512], in_=PS[1][:, :]),
                       reads=[PSB[1]], writes=[b_mbs])
                dma("sp", lambda e, q=q: e.dma_start(out=MODB[:, q * 2048:(q + 1) * 2048], in_=mbs[:]),
                    reads=[b_mbs], writes=[b_MODB])
            if dbg:
                Sx.finish([b_MODB], "sp")
        if os.environ.get("MK_STOP") == "1":
            with ExitStack() as ed:
                Sx.barrier()
                st = sb(ed, "dbgst0", [128, 2048], F32)
                b_st, b_do = Buf("dbgst0"), Buf("dbgo0")
                for q in range(12):
                    dma("sp", lambda e, q=q: e.dma_start(out=st[:], in_=MODB[:, q * 2048:(q + 1) * 2048]), reads=[b_MODB], writes=[b_st])
                    dma("sp", lambda e, q=q: e.dma_start(out=dbgout["d_mod"][:, q * 2048:(q + 1) * 2048], in_=st[:]), reads=[b_st], writes=[b_do])
                Sx.finish([b_do], "sp")
            build.sched = Sx
            return nc

        cast2d(flat(win_s, 0), flat(w_in, 0), (112 // NR) * 128 * KC * 128, b_win)
        cast2d(flat(wout_s, 0), flat(w_out, 0), (8 // NR) * 128 * KC * 512, b_wout)
        if NR > 1:
            ag_chunked(flat(win_s, 0), flat(win_b, 0), (112 // NR) * 128 * KC * 128 // 2048, [b_win], [b_win])
            ag_chunked(flat(wout_s, 0), flat(wout_b, 0), (8 // NR) * 128 * KC * 512 // 2048, [b_wout], [b_wout])

        def load_mod(dst, idx, q, bufs):
            dma(q, lambda e: e.dma_start(out=dst[:], in_=MODB[:, idx * D:(idx + 1) * D]), reads=[b_MODB], writes=bufs)

        small = sb(es, "small", [128, 8], F32)
        b_small = Buf("small")
        G = sb(es, "G", [128, NTB, NE], F32)
        b_G = Buf("G")
        ebig = ExitStack()
        big = sb(ebig, "big", [128, KC, S], BF16)
        b_big = Buf("big")

        def rstd_from_ss(ss_ap, out_ap, rd, wr):
            op("dve", lambda e: e.tensor_scalar(out=out_ap, in0=ss_ap, scalar1=1.0 / D, scalar2=EPS,
                                                op0=ALU.mult, op1=ALU.add), reads=rd, writes=wr)
            op("act", lambda e: e.activation(out=out_ap, in_=out_ap, func=AF.Sqrt), reads=wr, writes=wr)
            op("dve", lambda e: e.reciprocal(out=out_ap, in_=out_ap), reads=wr, writes=wr)

        with ExitStack() as e1:
            Sx.barrier()
            A1 = sb(e1, "A1", [128, D], F32)
            SH1 = sb(e1, "SH1", [128, D], F32)
            b_A1, b_SH1 = Buf("A1"), Buf("SH1")
            load_mod(SH1, 0, "sp", [b_SH1])
            load_mod(A1, 1, "act", [b_A1])
            xt = [sb(e1, "xt%d" % i, [128, D], F32) for i in range(2)]
            b_xt = [Buf("xt0"), Buf("xt1")]
            dma("sp", lambda e: e.dma_start(out=xt[1][:], in_=n_pre), writes=[b_xt[1]])
            op("dve", lambda e: e.scalar_tensor_tensor(out=A1[:], in0=A1[:], scalar=1.0, in1=xt[1][:],
                                                       op0=ALU.add, op1=ALU.mult), reads=[b_A1, b_xt[1]], writes=[b_A1])
            hb = sb(e1, "hb", [128, D], BF16)
            b_hb = Buf("hb")
            junk, b_junk = hb, b_hb
            for tb in range(NTB):
                i = tb % 2
                dma("sp" if i == 0 else "act", lambda e, tb=tb, i=i: e.dma_start(out=xt[i][:], in_=x[tb * 128:(tb + 1) * 128, :]),
                    writes=[b_xt[i]])
                op("act", lambda e, i=i: e.activation(out=junk[:], in_=xt[i][:], func=AF.Square, accum_out=small[:, 0:1]),
                   reads=[b_xt[i]], writes=[b_junk, b_small])
                rstd_from_ss(small[:, 0:1], small[:, 1:2], [b_small], [b_small])
                op("dve", lambda e, i=i: e.scalar_tensor_tensor(out=xt[i][:], in0=xt[i][:], scalar=small[:, 1:2], in1=A1[:],
                                                                 op0=ALU.mult, op1=ALU.mult),
                   reads=[b_xt[i], b_small, b_A1], writes=[b_xt[i]])
                op("pool", lambda e, i=i: e.tensor_tensor(out=hb[:], in0=xt[i][:], in1=SH1[:], op=ALU.add),
                   reads=[b_xt[i], b_SH1], writes=[b_hb])
                for g in range(4):
                    for j in range(8):
                        kc = g * 8 + j
                        op("pe", lambda e, kc=kc, j=j: e.transpose(out=PTR[:, j, :], in_=hb[:, kc * 128:(kc + 1) * 128], identity=ident_b),
                           reads=[b_hb, b_cb16], writes=[b_PTR], track=(j == 7))
                    op("act" if g % 2 == 0 else "dve",
                       lambda e, g=g, tb=tb: (e.copy if hasattr(e, "copy") else e.tensor_copy)(
                           out=big[:, g * 8:(g + 1) * 8, tb * 128:(tb + 1) * 128], in_=PTR[:, :, :]),
                       reads=[b_PTR], writes=[b_big])
        if dbg:
            with ExitStack() as ed:
                Sx.barrier()
                st = sb(ed, "dbgst", [128, 2048], F32)
                b_st = Buf("dbgst")
                b_do = Buf("dbgo")
                for kc in range(KC):
                    op("dve", lambda e, kc=kc: e.tensor_copy(out=st[:], in_=big[:, kc, :]), reads=[b_big], writes=[b_st])
                    dma("sp", lambda e, kc=kc: e.dma_start(out=dbgout["d_h1T"][:, kc, :], in_=st[:]), reads=[b_st], writes=[b_do])
                for q in range(12):
                    dma("sp", lambda e, q=q: e.dma_start(out=st[:], in_=MODB[:, q * 2048:(q + 1) * 2048]), reads=[b_MODB], writes=[b_st])
                    dma("sp", lambda e, q=q: e.dma_start(out=dbgout["d_mod"][:, q * 2048:(q + 1) * 2048], in_=st[:]), reads=[b_st], writes=[b_do])
                Sx.finish([b_do], "sp")

        if os.environ.get("MK_STOP") == "2":
            build.sched = Sx
            ebig.close()
            return nc
        b_OT = Buf("OT")
        with ExitStack() as e2:
            Sx.barrier()
            wblk = [sb(e2, "wblk%d" % i, [128, KC, 128], BF16) for i in range(2)]
            b_wblk = [Buf("wblk%d" % i) for i in range(2)]
            wctr = [0]

            def load_w(cb):
                i = wctr[0] % 2
                wctr[0] += 1
                dma("sp", lambda e: e.dma_start(out=wblk[i][:], in_=win_b[cb]), reads=[b_win], writes=[b_wblk[i]])
                return wblk[i], b_wblk[i]

            pctr = [0]

            def proj_fm(cb, evac):
                wt, wbuf = load_w(cb)
                for tc in range(4):
                    pi = pctr[0] % 2
                    pctr[0] += 1
                    for kc in range(KC):
                        op("pe", lambda e, kc=kc, tc=tc, pi=pi: e.matmul(PS[pi][:, :], lhsT=wt[:, kc, :], rhs=big[:, kc, tc * 512:(tc + 1) * 512],
                                                                        start=(kc == 0), stop=(kc == KC - 1)),
                           reads=[wbuf, b_big], writes=[PSB[pi]], track=(kc == KC - 1))
                    evac(tc, PS[pi], PSB[pi])

            def proj_tm(cb, dst, b_dst):
                wt, wbuf = load_w(cb)
                for g in range(4):
                    pi = pctr[0] % 2
                    pctr[0] += 1
                    for j in range(4):
                        tb = g * 4 + j
                        for kc in range(KC):
                            op("pe", lambda e, kc=kc, tb=tb, j=j, pi=pi: e.matmul(PS[pi][:, j * 128:(j + 1) * 128], lhsT=big[:, kc, tb * 128:(tb + 1) * 128],
                                                                                rhs=wt[:, kc, :], start=(kc == 0), stop=(kc == KC - 1)),
                               reads=[wbuf, b_big], writes=[PSB[pi]], track=(kc == KC - 1 and j == 3))
                    op("act", lambda e, g=g, pi=pi: e.copy(out=dst[:, g * 4:(g + 1) * 4, :], in_=PS[pi][:, :].rearrange("p (a b) -> p a b", a=4)),
                       reads=[PSB[pi]], writes=[b_dst])

            vt = sb(e2, "vt", [128, NTB, 128], BF16)
            b_vt = Buf("vt")
            oTt = sb(e2, "oTt", [128, S], BF16)
            b_oTt = Buf("oTt")

            with ExitStack() as e3:
                Sx.barrier()
                qT = sb(e3, "qT", [128, S], BF16)
                kT = sb(e3, "kT", [128, S], BF16)
                nkT = sb(e3, "nkT", [128, S], BF16)
                b_qT, b_kT, b_nkT = Buf("qT"), Buf("kT"), Buf("nkT")
                Et = [sb(e3, "Et%d" % i, [128, 512], F32) for i in range(2)]
                SPt = [sb(e3, "SPt%d" % i, [128, 512], F32) for i in range(2)]
                Rt = [sb(e3, "Rt%d" % i, [128, 512], F32) for i in range(2)]
                Wt = [sb(e3, "Wt%d" % i, [128, 512], BF16) for i in range(2)]
                b_E = [Buf("E0"), Buf("E1")]
                b_SP = [Buf("SP0"), Buf("SP1")]
                b_R = [Buf("R0"), Buf("R1")]
                b_W = [Buf("W0"), Buf("W1")]
                PZ, b_PZ = [PS[2], PS[3]], [PSB[2], PSB[3]]
                PC, b_PC = [PS[4], PS[5]], [PSB[4], PSB[5]]
                PAV, b_PAV = PS[6], PSB[6]
                it = [0]
                for hd in range(int(os.environ.get('MK_SB', 16))):
                    proj_fm(64 + hd, lambda tc, ps, pb: op("act", lambda e: e.copy(out=qT[:, tc * 512:(tc + 1) * 512], in_=ps[:, :]),
                                                           reads=[pb], writes=[b_qT]))

                    def evk(tc, ps, pb):
                        op("act", lambda e: e.copy(out=kT[:, tc * 512:(tc + 1) * 512], in_=ps[:, :]), reads=[pb], writes=[b_kT])
                        op("act", lambda e: e.activation(out=nkT[:, tc * 512:(tc + 1) * 512], in_=ps[:, :], func=AF.Copy, scale=-SCALE),
                           reads=[pb], writes=[b_nkT])
                    if not os.environ.get('MK_QONLY'):
                        proj_fm(80 + hd, evk)
                        if not os.environ.get('MK_KONLY'):
                            proj_tm(96 + hd, vt, b_vt)
                    for c in range(int(os.environ.get('MK_ATT', 4))):
                        t0 = c * 512
                        first = True
                        ri = 0
                        for jb in range(4 * c + 3, -1, -1):
                            k = it[0] % 2
                            it[0] += 1
                            off = max(0, jb * 128 - t0)
                            diag = jb >= 4 * c
                            cols = slice(off, 512)
                            qs = slice(t0 + off, t0 + 512)
                            ks = slice(jb * 128, (jb + 1) * 128)
                            op("pe", lambda e: e.matmul(PZ[k][:, cols], lhsT=kT[:, ks], rhs=qT[:, qs], start=True, stop=True),
                               reads=[b_kT, b_qT], writes=[b_PZ[k]])
                            op("act", lambda e: e.activation(out=Et[k][:, cols], in_=PZ[k][:, cols], func=AF.Exp, scale=SCALE),
                               reads=[b_PZ[k]], writes=[b_E[k]])
                            op("act", lambda e: e.activation(out=SPt[k][:, cols], in_=Et[k][:, cols], func=AF.Ln, bias=1.0),
                               reads=[b_E[k]], writes=[b_SP[k]])
                            if diag:
                                dc = slice(off, off + 128)
                                op("dve", lambda e: e.tensor_tensor(out=SPt[k][:, dc], in0=SPt[k][:, dc], in1=mask01, op=ALU.mult),
                                   reads=[b_SP[k], b_cst], writes=[b_SP[k]])
                            op("pe", lambda e: e.matmul(PC[k][:, cols], lhsT=utri, rhs=SPt[k][:, cols], start=True, stop=False),
                               reads=[b_SP[k], b_cst], writes=[b_PC[k]], track=False)
                            if not first:
                                rprev = 1 - ri
                                op("pe", lambda e: e.matmul(PC[k][:, cols], lhsT=ones_f, rhs=Rt[rprev][:, cols], start=False, stop=False),
                                   reads=[b_R[rprev], b_cst], writes=[b_PC[k]], track=False)
                            if diag:
                                dc = slice(off, off + 128)
                                op("pe", lambda e: e.matmul(PC[k][:, dc], lhsT=ident_b, rhs=bigm_b, start=False, stop=False),
                                   reads=[b_cb16], writes=[b_PC[k]], track=False)
                            op("pe", lambda e: e.matmul(PC[k][:, cols], lhsT=nkT[:, ks], rhs=qT[:, qs], start=False, stop=True),
                               reads=[b_nkT, b_qT], writes=[b_PC[k]])
                            if jb > 0:
                                if first:
                                    if off > 0:
                                        op("pool", lambda e: e.memset(Rt[ri][:, 0:off], 0.0), writes=[b_R[ri]])
                                    op("pool", lambda e: e.tensor_copy(out=Rt[ri][:, cols], in_=SPt[k][:, cols]),
                                       reads=[b_SP[k]], writes=[b_R[ri]])
                                else:
                                    rprev = 1 - ri
                                    if off > 0:
                                        op("pool", lambda e: e.tensor_copy(out=Rt[ri][:, 0:off], in_=Rt[rprev][:, 0:off]),
                                           reads=[b_R[rprev]], writes=[b_R[ri]])
                                    op("pool", lambda e: e.tensor_tensor(out=Rt[ri][:, cols], in0=Rt[rprev][:, cols], in1=SPt[k][:, cols], op=ALU.add),
                                       reads=[b_R[rprev], b_SP[k]], writes=[b_R[ri]])
                            if off > 0:
                                op("pool", lambda e: e.memset(Wt[k][:, 0:off], 0.0), writes=[b_W[k]])
                            op("act", lambda e: e.activation(out=Wt[k][:, cols], in_=PC[k][:, cols], func=AF.Exp, scale=-1.0),
                               reads=[b_PC[k]], writes=[b_W[k]])
                            op("pe", lambda e: e.matmul(PAV[:, :], lhsT=vt[:, jb, :], rhs=Wt[k][:, :], start=first, stop=(jb == 0)),
                               reads=[b_vt, b_W[k]], writes=[b_PAV], track=True)
                            first = False
                            ri = 1 - ri
                        op("dve", lambda e: e.tensor_copy(out=oTt[:, t0:t0 + 512], in_=PAV[:, :]), reads=[b_PAV], writes=[b_oTt])
                    dma("sp", lambda e: e.dma_start(out=OT[16 + hd], in_=oTt[:]), reads=[b_oTt], writes=[b_OT])

            with ExitStack() as e3:
                Sx.barrier()
                lbt = sb(e3, "lbt", [128, 2, 16], F32)
                lbv = sb(e3, "lbv", [128, 3, 16], F32)
                nwt = sb(e3, "nwt", [128, 1], F32)
                b_lb = Buf("lb")
                dma("sp", lambda e: e.dma_start(out=lbt[:], in_=hg_lb), writes=[b_lb])
                dma("sp", lambda e: e.dma_start(out=nwt[:], in_=hg_nw), writes=[b_lb])
                op("dve", lambda e: e.tensor_tensor(out=lbv[:, 2, :], in0=lbt[:, 0, :], in1=lbt[:, 1, :], op=ALU.subtract), reads=[b_lb], writes=[b_lb])
                op("act", lambda e: e.activation(out=lbv[:, 0, :], in_=lbv[:, 2, :], func=AF.Sigmoid), reads=[b_lb], writes=[b_lb])
                op("dve", lambda e: e.tensor_scalar(out=lbv[:, 1, :], in0=lbv[:, 0, :], scalar1=-1.0, scalar2=1.0, op0=ALU.mult, op1=ALU.add),
                   reads=[b_lb], writes=[b_lb])
                rmask = sb(e3, "rmask", [128, 512], BF16)
                b_rm = Buf("rmask")
                op("pool", lambda e: e.memset(rmask[:], 1.0), writes=[b_rm])
                op("pool", lambda e: e.memset(rmask[:, 0:512:64], 0.0), writes=[b_rm])
                Qb = sb(e3, "Qb", [128, S], BF16)
                Fb = sb(e3, "Fb", [128, S], BF16)
                Gb = sb(e3, "Gb", [128, S], BF16)
                b_Qb, b_Fb, b_Gb = Buf("Qb"), Buf("Fb"), Buf("Gb")
                FF = sb(e3, "FF", [128, 512], F32)
                LF = sb(e3, "LF", [128, 512], F32)
                Bt = sb(e3, "Bt", [128, 512], F32)
                EB = sb(e3, "EB", [128, 512], F32)
                qtl = sb(e3, "qtl", [128, 512], BF16)
                ktl = sb(e3, "ktl", [128, 512], BF16)
                ktm = sb(e3, "ktm", [128, 4, 128], BF16)
                ktm2 = sb(e3, "ktm2", [128, 4, 128], BF16)
                KVe = sb(e3, "KVe", [128, 8, 128], F32)
                Sall = sb(e3, "Sall", [128, 9, 128], F32)
                Sbf = sb(e3, "Sbf", [128, 8, 128], BF16)
                SCm = sb(e3, "SCm", [128, 4, 128], BF16)
                b_FF, b_LF, b_Bt, b_EB = Buf("FF"), Buf("LF"), Buf("Bt"), Buf("EB")
                b_qtl, b_ktl, b_ktm, b_KVe, b_Sall, b_Sbf, b_SCm = (Buf("qtl"), Buf("ktl"), Buf("ktm"), Buf("KVe"),
                                                                  Buf("Sall"), Buf("Sbf"), Buf("SCm"))
                for hd in range(int(os.environ.get('MK_HG', 16))):
                    proj_fm(hd, lambda tc, ps, pb: op("act", lambda e: e.copy(out=Qb[:, tc * 512:(tc + 1) * 512], in_=ps[:, :]),
                                                      reads=[pb], writes=[b_Qb]))
                    proj_fm(16 + hd, lambda tc, ps, pb: op("act", lambda e: e.activation(out=Fb[:, tc * 512:(tc + 1) * 512], in_=ps[:, :], func=AF.Sigmoid),
                                                           reads=[pb], writes=[b_Fb]))
                    proj_tm(32 + hd, vt, b_vt)
                    proj_fm(48 + hd, lambda tc, ps, pb: op("act", lambda e: e.activation(out=Gb[:, tc * 512:(tc + 1) * 512], in_=ps[:, :], func=AF.Silu),
                                                           reads=[pb], writes=[b_Gb]))
                    op("pool", lambda e: e.memset(Sall[:, 0, :], 0.0), writes=[b_Sall])
                    HGL = int(os.environ.get('MK_HGL', 99))

                    def quarter(qd):
                        qs = slice(qd * 512, (qd + 1) * 512)
                        if qd > 0:
                            op("dve", lambda e: e.tensor_copy(out=Sall[:, 0, :], in_=Sall[:, 8, :]), reads=[b_Sall], writes=[b_Sall])
                        op("dve", lambda e: e.tensor_scalar(out=FF[:], in0=Fb[:, qs], scalar1=lbv[:, 1, hd:hd + 1], scalar2=lbv[:, 0, hd:hd + 1],
                                                            op0=ALU.mult, op1=ALU.add), reads=[b_Fb, b_lb], writes=[b_FF])
                        op("act", lambda e: e.activation(out=LF[:], in_=FF[:], func=AF.Ln), reads=[b_FF], writes=[b_LF])
                        if HGL < 2:
                            return
                        op("dve", lambda e: e.tensor_tensor_scan(out=Bt[:], data0=rmask[:], data1=LF[:], initial=0.0, op0=ALU.mult, op1=ALU.add),
                           reads=[b_LF, b_rm], writes=[b_Bt])
                        if HGL < 3:
                            return
                        op("act", lambda e: e.activation(out=EB[:], in_=Bt[:], func=AF.Exp), reads=[b_Bt], writes=[b_EB])
                        op("dve", lambda e: e.tensor_tensor(out=qtl[:], in0=Qb[:, qs], in1=EB[:], op=ALU.mult), reads=[b_Qb, b_EB], writes=[b_qtl])
                        op("act", lambda e: e.activation(out=LF[:], in_=Bt[:], func=AF.Exp, scale=-1.0), reads=[b_Bt], writes=[b_LF])
                        op("dve", lambda e: e.tensor_scalar(out=FF[:], in0=FF[:], scalar1=-1.0, scalar2=1.0, op0=ALU.mult, op1=ALU.add),
                           reads=[b_FF], writes=[b_FF])
                        op("dve", lambda e: e.tensor_tensor(out=ktl[:], in0=FF[:], in1=LF[:], op=ALU.mult), reads=[b_FF, b_LF], writes=[b_ktl])
                        if HGL < 4:
                            op("pool", lambda e: e.tensor_copy(out=oTt[:, qs], in_=Bt[:]), reads=[b_Bt], writes=[b_oTt])
                            return
                        for j in range(4):
                            op("pe", lambda e: e.transpose(out=PTR[:, j, :], in_=ktl[:, j * 128:(j + 1) * 128], identity=ident_b),
                               reads=[b_ktl, b_cb16], writes=[b_PTR], track=(j == 3))
                        op("act", lambda e: e.activation(out=ktm[:, :, :], in_=PTR[:, 0:4, :], func=AF.Copy, scale=cst[:, 896:897]),
                           reads=[b_PTR, b_cst], writes=[b_ktm])
                        op("act", lambda e: e.activation(out=ktm2[:, :, :], in_=PTR[:, 0:4, :], func=AF.Copy, scale=cst[:, 897:898]),
                           reads=[b_PTR, b_cst], writes=[b_ktm])
                        if HGL < 5:
                            op("pool", lambda e: e.tensor_copy(out=oTt[:, qs], in_=ktl[:]), reads=[b_ktl], writes=[b_oTt])
                            return
                        for g in range(2):
                            pi = 2 + g
                            for j in range(4):
                                c = g * 4 + j
                                km = ktm if c % 2 == 0 else ktm2
                                op("pe", lambda e: e.matmul(PS[pi][:, j * 128:(j + 1) * 128], lhsT=km[:, c // 2, :], rhs=vt[:, qd * 4 + c // 2, :],
                                                            start=True, stop=True),
                                   reads=[b_ktm, b_vt], writes=[PSB[pi]], track=(j == 3))
                            for j in range(4):
                                c = g * 4 + j
                                op("act", lambda e: e.activation(out=KVe[:, c, :], in_=PS[pi][:, j * 128:(j + 1) * 128], func=AF.Copy,
                                                                 scale=EB[:, c * 64 + 63:c * 64 + 64]),
                                   reads=[PSB[pi], b_EB], writes=[b_KVe])
                        if HGL < 6:
                            return
                        for c in range(8):
                            op("dve", lambda e: e.scalar_tensor_tensor(out=Sall[:, c + 1, :], in0=Sall[:, c, :], scalar=EB[:, c * 64 + 63:c * 64 + 64],
                                                                       in1=KVe[:, c, :], op0=ALU.mult, op1=ALU.add),
                               reads=[b_Sall, b_EB, b_KVe], writes=[b_Sall])
                        op("pool", lambda e: e.tensor_copy(out=Sbf[:], in_=Sall[:, 0:8, :]), reads=[b_Sall], writes=[b_Sbf])
                        if HGL < 7:
                            return
                        for j in range(4):
                            bs = slice(j * 128, (j + 1) * 128)
                            op("pe", lambda e: e.matmul(PS[4][:, bs], lhsT=ktl[:, bs], rhs=qtl[:, bs], start=True, stop=True),
                               reads=[b_ktl, b_qtl], writes=[PSB[4]], track=(j == 3))
                        for j in range(4):
                            op("dve", lambda e: e.tensor_tensor(out=SCm[:, j, :], in0=PS[4][:, j * 128:(j + 1) * 128], in1=mask2, op=ALU.mult),
                               reads=[PSB[4], b_cst], writes=[b_SCm])
                        if HGL < 8:
                            return
                        for j in range(4):
                            oc = slice(j * 128, (j + 1) * 128)
                            op("pe", lambda e: e.matmul(PS[5][:, oc], lhsT=vt[:, qd * 4 + j, :], rhs=SCm[:, j, :], start=True, stop=False),
                               reads=[b_vt, b_SCm], writes=[PSB[5]], track=False)
                            for h in range(2):
                                c = j * 2 + h
                                occ = slice(c * 64, c * 64 + 64)
                                op("pe", lambda e: e.matmul(PS[5][:, occ], lhsT=Sbf[:, c, :], rhs=qtl[:, occ], start=False, stop=(h == 1)),
                                   reads=[b_Sbf, b_qtl], writes=[PSB[5]], track=(h == 1 and j == 3))
                        if HGL < 9:
                            return
                        op("act", lambda e: e.activation(out=LF[:], in_=PS[5][:, :], func=AF.Square), reads=[PSB[5], b_ktl], writes=[b_LF])
                        op("pe", lambda e: e.matmul(PS[6][:, :], lhsT=o128, rhs=LF[:], start=True, stop=True),
                           reads=[b_LF, b_cst], writes=[PSB[6]])
                        op("act", lambda e: e.activation(out=FF[:], in_=PS[6][:, :], func=AF.Sqrt, bias=EPS), reads=[PSB[6], b_ktl], writes=[b_FF])
                        op("dve", lambda e: e.reciprocal(out=FF[:], in_=FF[:]), reads=[b_FF], writes=[b_FF])
                        op("dve", lambda e: e.scalar_tensor_tensor(out=Bt[:], in0=PS[5][:, :], scalar=nwt[:, 0:1], in1=FF[:],
                                                                   op0=ALU.mult, op1=ALU.mult),
                           reads=[PSB[5], b_FF, b_lb, b_EB, b_qtl], writes=[b_Bt])
                        op("pool", lambda e: e.tensor_tensor(out=oTt[:, qs], in0=Bt[:], in1=Gb[:, qs], op=ALU.mult),
                           reads=[b_Bt, b_Gb], writes=[b_oTt])
                    for qd in range(4 if HGL > 0 else 0):
                        quarter(qd)
                    dma("sp", lambda e: e.dma_start(out=OT[hd], in_=oTt[:]), reads=[b_oTt], writes=[b_OT])

        if dbg:
            with ExitStack() as ed:
                Sx.barrier()
                st16 = sb(ed, "dbg16", [128, S], BF16)
                st = sb(ed, "dbgst2", [128, S], F32)
                b_st16, b_st, b_do = Buf("dbg16"), Buf("dbgst2"), Buf("dbgo2")
                for kc in range(32):
                    dma("sp", lambda e, kc=kc: e.dma_start(out=st16[:], in_=OT[kc]), reads=[b_OT], writes=[b_st16])
                    op("dve", lambda e: e.tensor_copy(out=st[:], in_=st16[:]), reads=[b_st16], writes=[b_st])
                    dma("sp", lambda e, kc=kc: e.dma_start(out=dbgout["d_oT"][kc], in_=st[:]), reads=[b_st], writes=[b_do])
                Sx.finish([b_do], "sp")

        if os.environ.get("MK_STOP") == "3":
            build.sched = Sx
            Sx.finish([b_OT, b_win, b_wout], "sp")
            ebig.close()
            return nc
        b_Y = Buf("Y")
        dma("sp", lambda e: e.dma_start(out=big[:], in_=OT.rearrange("k p s -> p k s")), reads=[b_OT], writes=[b_big])
        with ExitStack() as e4:
            Sx.barrier()
            wo = [sb(e4, "wo%d" % i, [128, KC, 512], BF16) for i in range(2)]
            b_wo = [Buf("wo0"), Buf("wo1")]
            yst = [sb(e4, "yst%d" % i, [128, 512], F32) for i in range(2)]
            b_yst = [Buf("yst0"), Buf("yst1")]
            n = 0
            for cg in range(8):
                i = cg % 2
                dma("act", lambda e, cg=cg, i=i: e.dma_start(out=wo[i][:], in_=wout_b[cg]), reads=[b_wout], writes=[b_wo[i]])
                for tb in range(NTB):
                    pi = n % 2
                    n += 1
                    for kc in range(KC):
                        op("pe", lambda e, kc=kc, tb=tb, i=i, pi=pi: e.matmul(PS[pi][:, :], lhsT=big[:, kc, tb * 128:(tb + 1) * 128], rhs=wo[i][:, kc, :],
                                                                            start=(kc == 0), stop=(kc == KC - 1)),
                           reads=[b_big, b_wo[i]], writes=[PSB[pi]], track=(kc == KC - 1))
                    op("act", lambda e, pi=pi: e.copy(out=yst[pi][:], in_=PS[pi][:, :]), reads=[PSB[pi]], writes=[b_yst[pi]])
                    dma("sp", lambda e, pi=pi, tb=tb, cg=cg: e.dma_start(out=Y[tb * 128:(tb + 1) * 128, cg * 512:(cg + 1) * 512], in_=yst[pi][:]),
                        reads=[b_yst[pi]], writes=[b_Y])

        if dbg and os.environ.get("MK_STOP") == "4":
            with ExitStack() as ed:
                Sx.barrier()
                st = sb(ed, "dbgst4", [128, D], F32)
                b_st, b_do = Buf("dbgst4"), Buf("dbgo4")
                for tb in range(NTB):
                    dma("sp", lambda e, tb=tb: e.dma_start(out=st[:], in_=Y[tb * 128:(tb + 1) * 128, :]), reads=[b_Y], writes=[b_st])
                    dma("sp", lambda e, tb=tb: e.dma_start(out=dbgout["d_y"][tb * 128:(tb + 1) * 128, :], in_=st[:]), reads=[b_st], writes=[b_do])
                Sx.finish([b_do], "sp")
            build.sched = Sx
            ebig.close()
            return nc
        ebig.close()
        b_X1, b_H2T = Buf("X1"), Buf("H2T")
        with ExitStack() as e5:
            Sx.barrier()
            G1W = sb(e5, "G1W", [128, D], F32)
            A2 = sb(e5, "A2", [128, D], F32)
            SH2 = sb(e5, "SH2", [128, D], F32)
            b_G1W, b_A2, b_SH2 = Buf("G1W"), Buf("A2"), Buf("SH2")
            ntmp = sb(e5, "ntmp", [128, D], F32)
            b_nt = Buf("ntmp")
            load_mod(G1W, 2, "sp", [b_G1W])
            dma("act", lambda e: e.dma_start(out=ntmp[:], in_=n_post), writes=[b_nt])
            op("dve", lambda e: e.tensor_tensor(out=G1W[:], in0=G1W[:], in1=ntmp[:], op=ALU.mult), reads=[b_G1W, b_nt], writes=[b_G1W])
            load_mod(A2, 4, "sp", [b_A2])
            dma("act", lambda e: e.dma_start(out=ntmp[:], in_=n_fpre), reads=[], writes=[b_nt])
            op("dve", lambda e: e.scalar_tensor_tensor(out=A2[:], in0=A2[:], scalar=1.0, in1=ntmp[:], op0=ALU.add, op1=ALU.mult),
               reads=[b_A2, b_nt], writes=[b_A2])
            load_mod(SH2, 3, "sp", [b_SH2])
            wrt = sb(e5, "wrt", [128, KC, NE], F32)
            brt = sb(e5, "brt", [128, NE], F32)
            b_wr = Buf("wr")
            dma("act", lambda e: e.dma_start(out=wrt[:], in_=w_r), writes=[b_wr])
            dma("act", lambda e: e.dma_start(out=brt[:], in_=b_r), writes=[b_wr])
            yt = sb(e5, "yt", [128, D], F32)
            xt2 = sb(e5, "xt2", [128, D], F32)
            h2f = sb(e5, "h2f", [128, D], F32)
            b_yt, b_xt2, b_h2f = Buf("yt"), Buf("xt2"), Buf("h2f")
            junk2 = sb(e5, "junk2", [128, D], BF16)
            b_j2 = Buf("junk2")
            h2Tb = sb(e5, "h2Tb", [128, KC, 128], BF16)
            loT = sb(e5, "loT", [128, KC, 128], BF16)
            hb2 = sb(e5, "hb2", [128, D], BF16)
            lo2 = sb(e5, "lo2", [128, D], BF16)
            whi = sb(e5, "whi", [128, KC, NE], BF16)
            wlo = sb(e5, "wlo", [128, KC, NE], BF16)
            b_h2Tb, b_loT, b_hb2, b_lo2 = Buf("h2Tb"), Buf("loT"), Buf("hb2"), Buf("lo2")
            op("dve", lambda e: e.tensor_copy(out=whi[:], in_=wrt[:]), reads=[b_wr], writes=[b_wr])
            op("dve", lambda e: e.tensor_tensor(out=wlo[:], in0=wrt[:], in1=whi[:], op=ALU.subtract), reads=[b_wr], writes=[b_wr])
            lg = sb(e5, "lg", [128, NE], F32)
            m8 = sb(e5, "m8", [128, 8], F32)
            ex = sb(e5, "ex", [128, NE], F32)
            mk = sb(e5, "mk", [128, NE], F32)
            b_lg = Buf("lg")
            D2L = int(os.environ.get('MK_D2L', 99))

            def d2blk(tb):
                rows = slice(tb * 128, (tb + 1) * 128)
                dma("sp", lambda e: e.dma_start(out=yt[:], in_=Y[rows, :]), reads=[b_Y], writes=[b_yt])
                dma("act", lambda e: e.dma_start(out=xt2[:], in_=x[rows, :]), writes=[b_xt2])
                op("act", lambda e: e.activation(out=junk2[:], in_=yt[:], func=AF.Square, accum_out=small[:, 2:3]),
                   reads=[b_yt], writes=[b_j2, b_small])
                rstd_from_ss(small[:, 2:3], small[:, 3:4], [b_small], [b_small])
                op("dve", lambda e: e.scalar_tensor_tensor(out=yt[:], in0=yt[:], scalar=small[:, 3:4], in1=G1W[:], op0=ALU.mult, op1=ALU.mult),
                   reads=[b_yt, b_small, b_G1W], writes=[b_yt])
                op("pool", lambda e: e.tensor_tensor(out=xt2[:], in0=xt2[:], in1=yt[:], op=ALU.add), reads=[b_xt2, b_yt], writes=[b_xt2])
                dma("sp", lambda e: e.dma_start(out=X1[rows, :], in_=xt2[:]), reads=[b_xt2], writes=[b_X1])
                if dbg:
                    dma("sp", lambda e: e.dma_start(out=dbgout["d_x1"][rows, :], in_=xt2[:]), reads=[b_xt2], writes=[b_X1])
                if D2L < 1:
                    return
                op("act", lambda e: e.activation(out=junk2[:], in_=xt2[:], func=AF.Square, accum_out=small[:, 2:3]),
                   reads=[b_xt2], writes=[b_j2, b_small])
                rstd_from_ss(small[:, 2:3], small[:, 3:4], [b_small], [b_small])
                if os.environ.get("MK_Y") != "nostt":
                    op("dve", lambda e: e.scalar_tensor_tensor(out=h2f[:], in0=xt2[:], scalar=small[:, 3:4], in1=A2[:], op0=ALU.mult, op1=ALU.mult),
                       reads=[b_xt2, b_small, b_A2], writes=[b_h2f])
                if os.environ.get("MK_X") != "nopool":
                    op("pool", lambda e: e.tensor_tensor(out=h2f[:], in0=h2f[:], in1=SH2[:], op=ALU.add), reads=[b_h2f, b_SH2], writes=[b_h2f])
                if D2L < 2:
                    return
                op("dve", lambda e: e.tensor_copy(out=hb2[:], in_=h2f[:]), reads=[b_h2f], writes=[b_hb2])
                op("pool", lambda e: e.tensor_tensor(out=lo2[:], in0=h2f[:], in1=hb2[:], op=ALU.subtract), reads=[b_h2f, b_hb2], writes=[b_lo2])
                for g in range(4):
                    for j in range(8):
                        kc = g * 8 + j
                        op("pe", lambda e: e.transpose(out=PTR[:, j, :], in_=hb2[:, kc * 128:(kc + 1) * 128], identity=ident_b),
                           reads=[b_hb2, b_cb16], writes=[b_PTR], track=(j == 7))
                    op("act", lambda e: e.copy(out=h2Tb[:, g * 8:(g + 1) * 8, :], in_=PTR[:, :, :]), reads=[b_PTR], writes=[b_h2Tb])
                for g in range(4):
                    for j in range(8):
                        kc = g * 8 + j
                        op("pe", lambda e: e.transpose(out=PTR[:, j, :], in_=lo2[:, kc * 128:(kc + 1) * 128], identity=ident_b),
                           reads=[b_lo2, b_cb16], writes=[b_PTR], track=(j == 7))
                    op("dve", lambda e: e.tensor_copy(out=loT[:, g * 8:(g + 1) * 8, :], in_=PTR[:, :, :]), reads=[b_PTR], writes=[b_loT])
                nmm = 0
                for (lt, bl, wt_, ) in ((h2Tb, b_h2Tb, whi), (h2Tb, b_h2Tb, wlo), (loT, b_loT, whi)):
                    for kc in range(KC):
                        nmm += 1
                        op("pe", lambda e: e.matmul(PS[2][:, 0:NE], lhsT=lt[:, kc, :], rhs=wt_[:, kc, :], start=(nmm == 1), stop=(nmm == 3 * KC)),
                           reads=[bl, b_wr], writes=[PSB[2]], track=(nmm == 3 * KC))
                if D2L < 3:
                    return
                dma("sp", lambda e, tb=tb: e.dma_start(out=H2T[:, :, tb * 128:(tb + 1) * 128], in_=h2Tb[:]), reads=[b_h2Tb], writes=[b_H2T])
                op("dve", lambda e: e.tensor_tensor(out=lg[:], in0=PS[2][:, 0:NE], in1=brt[:], op=ALU.add), reads=[PSB[2], b_wr], writes=[b_lg])
                if D2L < 4:
                    return
                op("dve", lambda e: e.max(out=m8[:], in_=lg[:]), reads=[b_lg], writes=[b_lg])
                op("dve", lambda e: e.tensor_scalar(out=m8[:, 5:6], in0=m8[:, 3:4], scalar1=-1.0, scalar2=0.0, op0=ALU.mult, op1=ALU.add), reads=[b_lg], writes=[b_lg])
                op("act", lambda e: e.activation(out=mk[:], in_=lg[:], func=AF.Identity, bias=m8[:, 5:6]), reads=[b_lg], writes=[b_lg])
                op("dve", lambda e: e.tensor_scalar(out=mk[:], in0=mk[:], scalar1=0.0, scalar2=0.0, op0=ALU.is_ge, op1=ALU.add), reads=[b_lg], writes=[b_lg])
                op("dve", lambda e: e.tensor_scalar(out=m8[:, 7:8], in0=m8[:, 0:1], scalar1=-1.0, scalar2=0.0, op0=ALU.mult, op1=ALU.add), reads=[b_lg], writes=[b_lg])
                if D2L < 5:
                    return
                op("act", lambda e: e.activation(out=ex[:], in_=lg[:], func=AF.Exp, bias=m8[:, 7:8]), reads=[b_lg], writes=[b_lg])
                op("dve", lambda e: e.tensor_tensor(out=ex[:], in0=ex[:], in1=mk[:], op=ALU.mult), reads=[b_lg], writes=[b_lg])
                op("dve", lambda e: e.tensor_reduce(out=m8[:, 6:7], in_=ex[:], axis=mybir.AxisListType.X, op=ALU.add), reads=[b_lg], writes=[b_lg])
                op("dve", lambda e: e.reciprocal(out=m8[:, 6:7], in_=m8[:, 6:7]), reads=[b_lg], writes=[b_lg])
                op("act", lambda e, tb=tb: e.activation(out=G[:, tb, :], in_=ex[:], func=AF.Copy, scale=m8[:, 6:7]),
                   reads=[b_lg], writes=[b_G])
            for tb in range(int(os.environ.get('MK_NTB', NTB))):
                d2blk(tb)
            if dbg:
                dma("sp", lambda e: e.dma_start(out=dbgout["d_G"], in_=G[:]), reads=[b_G], writes=[b_X1])
                Sx.finish([b_X1], "sp")

        build.sched = Sx
        if os.environ.get("MK_SKIP_MOE"):
            Sx.finish([b_X1, b_H2T, b_Y], "sp")
            return nc
        NEL = NE // NR
        b_chain = Buf("castchain")
        for i in range(NEL if NEXP == NE else max(1, NEXP // NR)):
            cast2d(flat(wgu_s[i], 0), flat(w_gu[i], 0), 24 * 128 * KC * 128, [b_wgu[i], b_chain])
            cast2d(flat(wdn_s[i], 0), flat(w_dn[i], 0), 8 * 128 * 12 * 512, [b_wdn[i], b_chain])
            if NR > 1:
                ag_chunked(flat(wgu_s[i], 0), flat(wgu_b[i], 0), 24 * 128 * KC * 128 // 2048, [b_wgu[i]], [b_wgu[i]])
                ag_chunked(flat(wdn_s[i], 0), flat(wdn_b[i], 0), 8 * 128 * 12 * 512 // 2048, [b_wdn[i]], [b_wdn[i]])
        experts = [(i, r) for i in range(NEL) for r in range(NR)][:NEXP]
        MOEL = int(os.environ.get('MK_MOEL', 99))

        def gu_src(i, r, cb):
            return wgu_b[i][r, cb] if NR > 1 else wgu_s[i][cb]

        def dn_src(i, r, cg):
            return wdn_b[i][r, cg] if NR > 1 else wdn_s[i][cg]

        with ExitStack() as e6:
            Sx.barrier()
            bgu = sb(e6, "bgu", [128, NE, 24], F32)
            bdn = sb(e6, "bdn", [NE, D], F32)
            GT = sb(e6, "GT", [NE, S], BF16)
            b_bias, b_GT = Buf("bias"), Buf("GT")
            dma("sp", lambda e: e.dma_start(out=bgu[:], in_=b_gu), writes=[b_bias])
            dma("sp", lambda e: e.dma_start(out=bdn[:], in_=b_dn), writes=[b_bias])
            Gb = sb(e6, "Gbf", [128, NTB, NE], BF16)
            bdnb = sb(e6, "bdnb", [NE, D], BF16)
            op("dve", lambda e: e.tensor_copy(out=Gb[:], in_=G[:]), reads=[b_G], writes=[b_GT])
            op("dve", lambda e: e.tensor_copy(out=bdnb[:], in_=bdn[:]), reads=[b_bias], writes=[b_bias])
            for g in range(2):
                for j in range(8):
                    tb = g * 8 + j
                    op("pe", lambda e: e.transpose(out=PTR[0:NE, j, :], in_=Gb[:, tb, :], identity=ident_b),
                       reads=[b_GT, b_cb16], writes=[b_PTR], track=(j == 7))
                op("act", lambda e: e.copy(out=GT[:, g * 1024:(g + 1) * 1024].rearrange("p (a b) -> p a b", a=8), in_=PTR[0:NE, :, :]),
                   reads=[b_PTR], writes=[b_GT])
            H = sb(e6, "H", [128, 2 * D], F32)
            b_h2g = Buf("h2g")
            h2g = H.bitcast(BF16)
            xf = H[:, 0:D]
            G2W = H[:, D:2 * D]
            b_xf = b_h2g
            b_G2W = b_h2g
            acc = sb(e6, "acc", [128, 4, D], F32)
            b_acc = [Buf("acc%d" % i) for i in range(4)]
            b_G2WD = Buf("G2WD")
            dma("sp", lambda e: e.dma_start(out=G2W, in_=MODB[:, 5 * D:6 * D]), reads=[b_MODB], writes=[b_h2g])
            dma("act", lambda e: e.dma_start(out=acc[:, 0, :], in_=n_fpost), writes=[b_acc[0]])
            op("dve", lambda e: e.tensor_tensor(out=G2W, in0=G2W, in1=acc[:, 0, :], op=ALU.mult), reads=[b_h2g, b_acc[0]], writes=[b_h2g])
            dma("sp", lambda e: e.dma_start(out=G2WD, in_=G2W), reads=[b_h2g], writes=[b_G2WD])
            wg = [sb(e6, "wg%d" % i, [128, KC, 128], BF16) for i in range(2)]
            b_wg = [Buf("wg%d" % i) for i in range(2)]
            wd = [sb(e6, "wd%d" % i, [128, 12, 512], BF16) for i in range(2)]
            b_wd = [Buf("wd0"), Buf("wd1")]
            GL = sb(e6, "GL", [128, 512], F32)
            SGm = sb(e6, "SGm", [128, 512], F32)
            LN = sb(e6, "LN", [128, 512], F32)
            actT = sb(e6, "actT", [128, 12, 512], BF16)
            b_GL, b_SGm, b_LN, b_actT = Buf("GL"), Buf("SGm"), Buf("LN"), Buf("actT")
            b_out = Buf("out")
            wgc, wdc, pc = [0], [0], [0]
            for tg in range(int(os.environ.get('MK_TG', 4)) if MOEL >= 2 else 0):
                dma("sp", lambda e, tg=tg: e.dma_start(out=h2g[:, :].rearrange("p (k t) -> p k t", k=KC), in_=H2T[:, :, tg * 512:(tg + 1) * 512]), reads=[b_H2T], writes=[b_h2g])
                for ei, (i, r) in enumerate(experts):
                    eid = r * NEL + i
                    for cbp in range(12):
                        for half in range(2):
                            cb = cbp + 12 * half
                            wi = wgc[0] % 2
                            wgc[0] += 1
                            dma("sp", lambda e, wi=wi, cb=cb: e.dma_start(out=wg[wi][:], in_=gu_src(i, r, cb)),
                                reads=[b_wgu[i]], writes=[b_wg[wi]])
                            pi = pc[0] % 2
                            pc[0] += 1
                            for kc in range(KC):
                                op("pe", lambda e, kc=kc, wi=wi, pi=pi: e.matmul(PS[pi][:, :], lhsT=wg[wi][:, kc, :], rhs=h2g[:, kc * 512:(kc + 1) * 512],
                                                                                start=(kc == 0), stop=(kc == KC - 1)),
                                   reads=[b_wg[wi], b_h2g], writes=[PSB[pi]], track=(kc == KC - 1))
                            bcol = bgu[:, eid, cb:cb + 1]
                            if half == 0:
                                op("act", lambda e, pi=pi, bcol=bcol: e.activation(out=GL[:], in_=PS[pi][:, :], func=AF.Identity, bias=bcol),
                                   reads=[PSB[pi], b_bias], writes=[b_GL])
                                op("dve", lambda e: e.tensor_scalar(out=GL[:], in0=GL[:], scalar1=7.0, scalar2=0.0, op0=ALU.min, op1=ALU.add),
                                   reads=[b_GL], writes=[b_GL])
                                op("act", lambda e: e.activation(out=SGm[:], in_=GL[:], func=AF.Sigmoid, scale=1.702), reads=[b_GL], writes=[b_SGm])
                                op("pool", lambda e: e.tensor_tensor(out=GL[:], in0=GL[:], in1=SGm[:], op=ALU.mult), reads=[b_GL, b_SGm], writes=[b_GL])
                            else:
                                op("act", lambda e, pi=pi, bcol=bcol: e.activation(out=LN[:], in_=PS[pi][:, :], func=AF.Identity, bias=bcol),
                                   reads=[PSB[pi], b_bias], writes=[b_LN])
                                op("dve", lambda e: e.tensor_scalar(out=LN[:], in0=LN[:], scalar1=7.0, scalar2=-7.0, op0=ALU.min, op1=ALU.max),
                                   reads=[b_LN], writes=[b_LN])
                                op("pool", lambda e: e.tensor_scalar(out=LN[:], in0=LN[:], scalar1=1.0, scalar2=1.0, op0=ALU.mult, op1=ALU.add),
                                   reads=[b_LN], writes=[b_LN])
                                op("dve", lambda e, cbp=cbp: e.tensor_tensor(out=actT[:, cbp, :], in0=GL[:], in1=LN[:], op=ALU.mult),
                                   reads=[b_GL, b_LN], writes=[b_actT])
                    for cg in range(8 if MOEL >= 3 else 0):
                        di_ = wdc[0] % 2
                        wdc[0] += 1
                        dma("act", lambda e, di_=di_, cg=cg: e.dma_start(out=wd[di_][:], in_=dn_src(i, r, cg)), reads=[b_wdn[i]], writes=[b_wd[di_]])
                        for t4 in range(4):
                            pi = 2 + pc[0] % 2
                            pc[0] += 1
                            for kc in range(12):
                                op("pe", lambda e, kc=kc, t4=t4, di_=di_, pi=pi: e.matmul(PS[pi][:, :], lhsT=actT[:, kc, t4 * 128:(t4 + 1) * 128], rhs=wd[di_][:, kc, :],
                                                                                        start=(kc == 0), stop=(kc == 11)),
                                   reads=[b_actT, b_wd[di_]], writes=[PSB[pi]], track=(kc == 11))
                            gcol = G[:, tg * 4 + t4, eid:eid + 1]
                            ac = acc[:, t4, cg * 512:(cg + 1) * 512]
                            if ei == 0:
                                op("act", lambda e, pi=pi, gcol=gcol, ac=ac: e.activation(out=ac, in_=PS[pi][:, :], func=AF.Copy, scale=gcol),
                                   reads=[PSB[pi], b_G], writes=[b_acc[t4]])
                            else:
                                op("dve", lambda e, pi=pi, gcol=gcol, ac=ac: e.scalar_tensor_tensor(out=ac, in0=PS[pi][:, :], scalar=gcol, in1=ac, op0=ALU.mult, op1=ALU.add),
                                   reads=[PSB[pi], b_G, b_acc[t4]], writes=[b_acc[t4]])
                for t4 in range(4 if MOEL >= 4 else 0):
                    tb = tg * 4 + t4
                    rows = slice(tb * 128, (tb + 1) * 128)
                    for cg in range(8):
                        pi = 4 + cg % 2
                        op("pe", lambda e, cg=cg, tb=tb, pi=pi: e.matmul(PS[pi][:, :], lhsT=GT[:, tb * 128:(tb + 1) * 128], rhs=bdnb[:, cg * 512:(cg + 1) * 512],
                                                                        start=True, stop=True), reads=[b_GT, b_bias], writes=[PSB[pi]])
                        ac = acc[:, t4, cg * 512:(cg + 1) * 512]
                        op("dve", lambda e, pi=pi, ac=ac: e.tensor_tensor(out=ac, in0=PS[pi][:, :], in1=ac, op=ALU.add), reads=[PSB[pi], b_acc[t4]], writes=[b_acc[t4]])
                    if t4 == 0:
                        dma("act", lambda e: e.dma_start(out=G2W, in_=G2WD), reads=[b_G2WD], writes=[b_h2g])
                    op("act", lambda e, t4=t4: e.activation(out=xf, in_=acc[:, t4, :], func=AF.Square, accum_out=small[:, 6:7]),
                       reads=[b_acc[t4]], writes=[b_xf, b_small])
                    dma("sp", lambda e, rows=rows: e.dma_start(out=xf, in_=X1[rows, :]), reads=[b_X1, b_out], writes=[b_xf])
                    rstd_from_ss(small[:, 6:7], small[:, 7:8], [b_small], [b_small])
                    op("dve", lambda e, t4=t4: e.scalar_tensor_tensor(out=acc[:, t4, :], in0=acc[:, t4, :], scalar=small[:, 7:8], in1=G2W, op0=ALU.mult, op1=ALU.mult),
                       reads=[b_acc[t4], b_small, b_G2W], writes=[b_acc[t4]])
                    op("pool", lambda e, t4=t4: e.tensor_tensor(out=xf, in0=xf, in1=acc[:, t4, :], op=ALU.add), reads=[b_xf, b_acc[t4]], writes=[b_xf])
                    dma("sp", lambda e, rows=rows: e.dma_start(out=out[rows, :], in_=xf), reads=[b_xf], writes=[b_out])
            Sx.barrier()
            Sx.finish([b_out], "sp")
    return nc


def make_consts():
    c = np.zeros((128, 1152), np.float32)
    i = np.arange(128)
    c[:, 0:128] = np.eye(128)
    c[:, 128:256] = (i[:, None] < i[None, :])
    c[:, 256:384] = (i[:, None] >= i[None, :])
    c[:, 384:512] = 1.0
    c[:, 512:640] = 30000.0 * (i[:, None] >= i[None, :])
    c[:, 640:768] = ((i[:, None] // 64) == (i[None, :] // 64)) & (i[:, None] <= i[None, :])
    c[:, 768:896] = 1.0 / 128
    c[:, 896] = (i < 64)
    c[:, 897] = (i >= 64)
    return c


def core_inputs(inp, b, r, NR):
    KCS = KC // NR
    f = np.ascontiguousarray
    d = {}
    d["x"] = f(inp["x"][b])
    k0 = r * KCS * 128
    cs = inp["c"][:, k0:k0 + KCS * 128]
    d["cT"] = f(cs.reshape(NB, KCS, 128).transpose(2, 1, 0))
    d["w_mod"] = f(inp["w_mod"][0][k0:k0 + KCS * 128])
    d["b_mod"] = f(inp["b_mod"][0:1])
    oh = np.zeros((NR * NB + 1, 128), np.float32)
    for rr in range(NR):
        oh[rr * NB + b] = 1.0
    oh[NR * NB] = 1.0
    d["oh"] = oh
    rep = lambda v: f(np.broadcast_to(v.reshape(1, -1), (128, v.size)))
    d["n_pre"] = rep(inp["mix_pre_norm"][0])
    d["n_post"] = rep(inp["mix_post_norm"][0])
    d["n_fpre"] = rep(inp["ffn_pre_norm"][0])
    d["n_fpost"] = rep(inp["ffn_post_norm"][0])
    ncb = 112 // NR
    wi = inp["w_in"][0][:, r * ncb * 128:(r + 1) * ncb * 128]
    d["w_in"] = f(wi.reshape(KC, 128, ncb, 128).transpose(2, 1, 0, 3))
    ncg = 8 // NR
    wo = inp["w_out"][0][:, r * ncg * 512:(r + 1) * ncg * 512]
    d["w_out"] = f(wo.reshape(KC, 128, ncg, 512).transpose(2, 1, 0, 3))
    d["hg_lb"] = f(inp["hg_lb"].reshape(2, 16, 128).transpose(2, 0, 1))
    d["hg_nw"] = f(inp["hg_norm_w"][0].reshape(128, 1))
    d["w_r"] = f(inp["w_router"][0].reshape(KC, 128, NE).transpose(1, 0, 2))
    d["b_r"] = rep(inp["b_router"][0])
    nel = NE // NR
    nexp_ = int(os.environ.get("MK_NEXP", NE))
    nld_ = 1 if os.environ.get("MK_SKIP_MOE") else (nel if nexp_ == NE else max(1, nexp_ // NR))
    wg = inp["w_gu"][0][r * nel:r * nel + nld_]
    d["w_gu"] = f(wg.reshape(wg.shape[0], KC, 128, 24, 128).transpose(0, 3, 2, 1, 4))
    d["b_gu"] = f(inp["b_gu"][0].reshape(NE, 24, 128).transpose(2, 0, 1))
    wd = inp["w_down"][0][r * nel:r * nel + nld_]
    d["w_dn"] = f(wd.reshape(wd.shape[0], 12, 128, 8, 512).transpose(0, 3, 2, 1, 4))
    d["b_dn"] = f(inp["b_down"][0])
    d["consts"] = make_consts()
    return d


def kernel(**inputs):
    inp = {k: np.asarray(v) for k, v in inputs.items()}
    NR = 4
    nc = build(NR)
    in_maps = [core_inputs(inp, b, b % NR, NR) for b in range(8)]
    res = run_bass_kernel_spmd(nc, in_maps, core_ids=list(range(8)))
    return np.stack([res.results[b]["out"] for b in range(8)], axis=0).astype(np.float32)
```
